# Optimizing a Trainium2 kernel written in Bass

```python
import math
import jax, jax.numpy as jnp
from jax import lax
import numpy as np

D_MODEL = 2048
BATCH = 4
SEQ = 2048
DEPTH = 2

CHUNK = 64
N_META = 16
D_FF = 4 * D_MODEL
MIX_WIDTH = D_MODEL
POOL_WIDTH = MIX_WIDTH // 2
POOL_WINDOWS = (2, 4, 8, 16)
POOL_GROUP = POOL_WIDTH // len(POOL_WINDOWS)
SSM_WIDTH = MIX_WIDTH - POOL_WIDTH
SSM_GROUP_CH = 16
SSM_GROUPS = SSM_WIDTH // SSM_GROUP_CH
SSM_STATE = 64
DT_MIN = 0.001
DT_MAX = 0.1
HEAD_DIM = 128
N_HEADS = D_MODEL // HEAD_DIM
N_KV_HEADS = 4
KV_GROUP = N_HEADS // N_KV_HEADS
IDX_HEADS = 16
IDX_DIM = 64
ROPE_THETA = 500000.0
ROPE_FRAC = 4
TOPK_MAX = 256
Q_BLOCK = 128
EPS = 1e-6
N_EVEN = (DEPTH + 1) // 2
N_ODD = DEPTH // 2
Q_W = N_HEADS * HEAD_DIM
KV_W = N_KV_HEADS * HEAD_DIM
IQ_W = IDX_HEADS * IDX_DIM
C_IN_WIDTH = Q_W + 2 * KV_W + IQ_W + IDX_DIM + IDX_HEADS

kernel_name = 'hybrid_pool_s5_dsa_block'


def rms_norm(x, g):
    xf = x.astype(jnp.float32)
    y = xf * lax.rsqrt(jnp.mean(xf * xf, axis=-1, keepdims=True) + EPS)
    return (y * g.astype(jnp.float32)).astype(x.dtype)


def chunk_ids(T):
    t = jnp.arange(T)
    return jnp.where(t < N_META, 0, (t - N_META) // CHUNK + 1)


def rope_partial(x, pos):
    d = x.shape[-1]
    r = d // ROPE_FRAC
    half = r // 2
    inv = ROPE_THETA ** (-jnp.arange(half, dtype=jnp.float32) / half)
    ang = pos.astype(jnp.float32)[:, None] * inv[None, :]
    cos = jnp.cos(ang)[:, None, :]
    sin = jnp.sin(ang)[:, None, :]
    x1 = x[..., :half].astype(jnp.float32)
    x2 = x[..., half:r].astype(jnp.float32)
    rot = jnp.concatenate([x1 * cos - x2 * sin, x2 * cos + x1 * sin], axis=-1).astype(x.dtype)
    return jnp.concatenate([rot, x[..., r:]], axis=-1)


def pool_mixer(u, w, scale):
    T = u.shape[1]
    uf = u.astype(jnp.float32)
    cs = jnp.pad(jnp.cumsum(uf, axis=1), ((0, 0), (1, 0), (0, 0)))
    count = jnp.arange(1, T + 1, dtype=jnp.float32)[None, :, None]
    outs = []
    for g, win in enumerate(POOL_WINDOWS):
        sl = slice(g * POOL_GROUP, (g + 1) * POOL_GROUP)
        c = cs[..., sl]
        lower = jnp.pad(c, ((0, 0), (win, 0), (0, 0)))[:, 1:T + 1]
        mean = (c[:, 1:] - lower) / jnp.minimum(count, float(win))
        diff = (mean - uf[..., sl]).astype(u.dtype)
        outs.append(diff @ w[g])
    return jnp.concatenate(outs, axis=-1) * scale


def _ssm_combine(e1, e2):
    a1r, a1i, b1r, b1i = e1
    a2r, a2i, b2r, b2i = e2
    return (a2r * a1r - a2i * a1i,
            a2r * a1i + a2i * a1r,
            a2r * b1r - a2i * b1i + b2r,
            a2r * b1i + a2i * b1r + b2i)


def s5_mixer(u, lam_re, lam_im, log_dt, b_re, b_im, c_re, c_im, d, w_glu):
    Bsz, T, _ = u.shape
    f32 = jnp.float32
    uf = u.astype(f32).reshape(Bsz, T, SSM_GROUPS, SSM_GROUP_CH)
    dt = jnp.exp(log_dt.astype(f32))[:, None]
    ar = lam_re.astype(f32)
    ai = lam_im.astype(f32)
    mag = jnp.exp(dt * ar)
    abar_re = mag * jnp.cos(dt * ai)
    abar_im = mag * jnp.sin(dt * ai)
    den = ar * ar + ai * ai
    zr = abar_re - 1.0
    zi = abar_im
    f_re = (zr * ar + zi * ai) / den
    f_im = (zi * ar - zr * ai) / den
    br = b_re.astype(f32)
    bi = b_im.astype(f32)
    bb_re = f_re[..., None] * br - f_im[..., None] * bi
    bb_im = f_re[..., None] * bi + f_im[..., None] * br
    bu_re = jnp.einsum('btgc,gnc->btgn', uf, bb_re)
    bu_im = jnp.einsum('btgc,gnc->btgn', uf, bb_im)
    a_re = jnp.broadcast_to(abar_re, bu_re.shape)
    a_im = jnp.broadcast_to(abar_im, bu_re.shape)
    _, _, h_re, h_im = lax.associative_scan(_ssm_combine, (a_re, a_im, bu_re, bu_im), axis=1)
    y = (jnp.einsum('btgn,gcn->btgc', h_re, c_re.astype(f32))
         - jnp.einsum('btgn,gcn->btgc', h_im, c_im.astype(f32))
         + d.astype(f32) * uf)
    y = jax.nn.gelu(y.reshape(Bsz, T, SSM_WIDTH)).astype(u.dtype)
    return y * jax.nn.sigmoid(y @ w_glu)


def dsa_mixer(h, w_in, w_out):
    Bsz, T, _ = h.shape
    f32 = jnp.float32
    kk = min(TOPK_MAX, (T - N_META) // 4)
    proj = h @ w_in
    o = 0
    q = proj[..., o:o + Q_W].reshape(Bsz, T, N_HEADS, HEAD_DIM); o += Q_W
    k = proj[..., o:o + KV_W].reshape(Bsz, T, N_KV_HEADS, HEAD_DIM); o += KV_W
    v = proj[..., o:o + KV_W].reshape(Bsz, T, N_KV_HEADS, HEAD_DIM); o += KV_W
    qi = proj[..., o:o + IQ_W].reshape(Bsz, T, IDX_HEADS, IDX_DIM); o += IQ_W
    ki = proj[..., o:o + IDX_DIM]; o += IDX_DIM
    wi = proj[..., o:o + IDX_HEADS]
    pos = jnp.arange(T)
    q = rope_partial(q, pos)
    k = rope_partial(k, pos)
    qi = rope_partial(qi, pos)
    ki = rope_partial(ki[:, :, None, :], pos)[:, :, 0, :].astype(f32)
    cid = chunk_ids(T)
    nb = -(-T // Q_BLOCK)
    pad = nb * Q_BLOCK - T

    def to_blocks(a):
        a = jnp.pad(a, [(0, 0), (0, pad)] + [(0, 0)] * (a.ndim - 2))
        return a.reshape((Bsz, nb, Q_BLOCK) + a.shape[2:]).swapaxes(0, 1)

    cid_q = jnp.pad(cid, (0, pad), mode='edge').reshape(nb, Q_BLOCK)
    w_scale = (IDX_HEADS ** -0.5) * (IDX_DIM ** -0.5)
    att_scale = HEAD_DIM ** -0.5

    def block(args):
        qb, qib, wib, cq = args
        s_idx = jax.nn.relu(jnp.einsum('bqhd,bsd->bqhs', qib.astype(f32), ki))
        score = jnp.einsum('bqh,bqhs->bqs', wib.astype(f32) * w_scale, s_idx)
        allowed = cid[None, :] <= cq[:, None]
        score = jnp.where(allowed[None], score, -jnp.inf)
        _, idx = lax.top_k(score, kk)
        valid = cid[idx] <= cq[None, :, None]
        k_sel = jax.vmap(lambda kb, ib: kb[ib])(k, idx)
        v_sel = jax.vmap(lambda vb, ib: vb[ib])(v, idx)
        qg = qb.reshape(Bsz, Q_BLOCK, N_KV_HEADS, KV_GROUP, HEAD_DIM)
        logits = jnp.einsum('bqhgd,bqshd->bqhgs', qg, k_sel).astype(f32) * att_scale
        logits = jnp.where(valid[:, :, None, None, :], logits, -jnp.inf)
        p = jax.nn.softmax(logits, axis=-1).astype(v.dtype)
        ob = jnp.einsum('bqhgs,bqshd->bqhgd', p, v_sel)
        return ob.reshape(Bsz, Q_BLOCK, Q_W)

    out = lax.map(block, (to_blocks(q), to_blocks(qi), to_blocks(wi), cid_q))
    out = out.swapaxes(0, 1).reshape(Bsz, nb * Q_BLOCK, Q_W)[:, :T]
    return out @ w_out


def setup_inputs(seed: int = 0) -> dict:
    key = jax.random.key(seed)
    ks = jax.random.split(key, 24)
    f32 = jnp.float32

    def nrm(k, shape, s):
        return jax.random.normal(k, shape, f32) * s

    n_idx = jnp.arange(SSM_STATE, dtype=f32)
    return {
        'x': nrm(ks[0], (BATCH, SEQ, D_MODEL), 1.0),
        'meta': nrm(ks[1], (N_META, D_MODEL), 1.0),
        'norm_g': 1.0 + nrm(ks[2], (DEPTH, 4, D_MODEL), 0.02),
        'ab_w_in': nrm(ks[3], (N_EVEN, D_MODEL, MIX_WIDTH), D_MODEL ** -0.5),
        'pool_w': nrm(ks[4], (N_EVEN, len(POOL_WINDOWS), POOL_GROUP, POOL_GROUP), POOL_GROUP ** -0.5),
        'pool_scale': 1.0 + nrm(ks[5], (N_EVEN, POOL_WIDTH), 0.02),
        's5_lambda_re': -0.5 + nrm(ks[6], (N_EVEN, SSM_GROUPS, SSM_STATE), 0.01),
        's5_lambda_im': math.pi * n_idx + nrm(ks[7], (N_EVEN, SSM_GROUPS, SSM_STATE), 0.01),
        's5_log_dt': jax.random.uniform(ks[8], (N_EVEN, SSM_GROUPS), f32, math.log(DT_MIN), math.log(DT_MAX)),
        's5_b_re': nrm(ks[9], (N_EVEN, SSM_GROUPS, SSM_STATE, SSM_GROUP_CH), (2.0 * SSM_GROUP_CH) ** -0.5),
        's5_b_im': nrm(ks[10], (N_EVEN, SSM_GROUPS, SSM_STATE, SSM_GROUP_CH), (2.0 * SSM_GROUP_CH) ** -0.5),
        's5_c_re': nrm(ks[11], (N_EVEN, SSM_GROUPS, SSM_GROUP_CH, SSM_STATE), (2.0 * SSM_STATE) ** -0.5),
        's5_c_im': nrm(ks[12], (N_EVEN, SSM_GROUPS, SSM_GROUP_CH, SSM_STATE), (2.0 * SSM_STATE) ** -0.5),
        's5_d': nrm(ks[13], (N_EVEN, SSM_GROUPS, SSM_GROUP_CH), 1.0),
        's5_w_glu': nrm(ks[14], (N_EVEN, SSM_WIDTH, SSM_WIDTH), SSM_WIDTH ** -0.5),
        'ab_w_out': nrm(ks[15], (N_EVEN, MIX_WIDTH, D_MODEL), MIX_WIDTH ** -0.5),
        'c_w_in': nrm(ks[16], (N_ODD, D_MODEL, C_IN_WIDTH), D_MODEL ** -0.5),
        'c_w_out': nrm(ks[17], (N_ODD, Q_W, D_MODEL), Q_W ** -0.5),
        'mlp_w1': nrm(ks[18], (DEPTH, D_MODEL, D_FF), D_MODEL ** -0.5),
        'mlp_w2': nrm(ks[19], (DEPTH, D_FF, D_MODEL), D_FF ** -0.5),
    }


def reference(x, meta, norm_g, ab_w_in, pool_w, pool_scale, s5_lambda_re, s5_lambda_im, s5_log_dt,
              s5_b_re, s5_b_im, s5_c_re, s5_c_im, s5_d, s5_w_glu, ab_w_out, c_w_in, c_w_out,
              mlp_w1, mlp_w2):
    Bsz = x.shape[0]
    meta_b = jnp.broadcast_to(meta[None].astype(x.dtype), (Bsz, N_META, x.shape[-1]))
    h = jnp.concatenate([meta_b, x], axis=1)
    for layer in range(DEPTH):
        g = norm_g[layer]
        a = rms_norm(h, g[0])
        if layer % 2 == 0:
            e = layer // 2
            u = a @ ab_w_in[e]
            y_pool = pool_mixer(u[..., :POOL_WIDTH], pool_w[e], pool_scale[e])
            y_ssm = s5_mixer(u[..., POOL_WIDTH:], s5_lambda_re[e], s5_lambda_im[e], s5_log_dt[e],
                             s5_b_re[e], s5_b_im[e], s5_c_re[e], s5_c_im[e], s5_d[e], s5_w_glu[e])
            mix = jnp.concatenate([y_pool, y_ssm], axis=-1) @ ab_w_out[e]
        else:
            o = layer // 2
            mix = dsa_mixer(a, c_w_in[o], c_w_out[o])
        h = h + rms_norm(mix, g[1])
        a = rms_norm(h, g[2])
        f = jnp.square(jax.nn.relu(a @ mlp_w1[layer])) @ mlp_w2[layer]
        h = h + rms_norm(f, g[3])
    return h[:, N_META:]
```

```python
import math, os
from contextlib import ExitStack
import numpy as np
import ml_dtypes
import concourse.bass as bass
import concourse.mybir as mybir
from concourse.bass_utils import run_bass_kernel_spmd

F32 = mybir.dt.float32
BF16 = mybir.dt.bfloat16
ALU = mybir.AluOpType
AF = mybir.ActivationFunctionType
AX = mybir.AxisListType

D = 2048
DFF = 8192
SEQ = 2048
NMETA = 16
T = SEQ + NMETA
NT = 1040
EPS = 1e-6
KC = D // 128


class Prog:
    def __init__(self, nc, es):
        self.nc = nc
        self.es = es
        self.engs = {"pe": nc.tensor, "act": nc.scalar, "dve": nc.vector,
                     "pool": nc.gpsimd, "sp": nc.sync}
        self.esem = {}
        self.ecnt = {}
        self.semobj = {}
        for e in self.engs:
            self.esem[e] = es.enter_context(nc.semaphore("s_" + e))
            self.ecnt[e] = 0
            self.semobj["e:" + e] = self.esem[e]
        self.known = {e: {} for e in self.engs}
        self.res = {}
        self.dsem = {}
        self.dcnt = {}
        self.n_inst = 0

    def sb(self, es, name, shape, dt=F32):
        return es.enter_context(self.nc.sbuf_tensor(name, list(shape), dt))

    def psum(self, es, name, shape, dt=F32):
        return es.enter_context(self.nc.psum_tensor(name, list(shape), dt))

    def _dma_sem(self, key):
        if key not in self.dsem:
            s = self.es.enter_context(self.nc.semaphore("d_" + key))
            self.dsem[key] = s
            self.dcnt[key] = 0
            self.semobj["d:" + key] = s
        return "d:" + key

    def _wait(self, eng, semkey, val):
        k = self.known[eng]
        if k.get(semkey, 0) >= val:
            return
        self.engs[eng].wait_ge(self.semobj[semkey], val)
        k[semkey] = val

    def _deps(self, eng, reads, writes):
        for r in reads:
            st = self.res.get(r)
            if st and st["w"]:
                self._wait(eng, *st["w"])
        for w in writes:
            st = self.res.get(w)
            if st:
                if st["w"]:
                    self._wait(eng, *st["w"])
                for sk, v in st["r"].items():
                    self._wait(eng, sk, v)

    def _record(self, tok, reads, writes):
        for r in reads:
            st = self.res.setdefault(r, {"w": None, "r": {}})
            st["r"][tok[0]] = max(st["r"].get(tok[0], 0), tok[1])
        for w in writes:
            self.res[w] = {"w": tok, "r": {}}

    def op(self, eng, fn, reads=(), writes=(), acc=False):
        if acc:
            self._deps(eng, reads, ())
        else:
            self._deps(eng, reads, writes)
        inst = fn()
        self.ecnt[eng] += 1
        inst.then_inc(self.esem[eng], 1)
        tok = ("e:" + eng, self.ecnt[eng])
        self._record(tok, reads, writes)
        self.n_inst += 1
        return inst

    def dma(self, eng, out, in_, key, reads=(), writes=(), **kw):
        sk = self._dma_sem(key)
        self._deps(eng, reads, writes)
        inst = self.engs[eng].dma_start(out=out, in_=in_, **kw)
        self.dcnt[key] += 16
        inst.then_inc(self.dsem[key], 16)
        tok = (sk, self.dcnt[key])
        self._record(tok, reads, writes)
        self.n_inst += 1
        return inst

    def barrier(self):
        for e in self.engs:
            for o in self.engs:
                if self.ecnt[o] > 0:
                    self._wait(e, "e:" + o, self.ecnt[o])
            for key, c in self.dcnt.items():
                if c > 0:
                    self._wait(e, "d:" + key, c)
        self.res = {}

    def mm(self, out, lhsT, rhs, start, stop, reads, writes):
        nc = self.nc
        return self.op("pe", lambda: nc.tensor.matmul(out, lhsT=lhsT, rhs=rhs, start=start, stop=stop),
                       reads=reads, writes=writes, acc=not start)

    def transpose(self, out, in_, ident, reads, writes):
        nc = self.nc
        return self.op("pe", lambda: nc.tensor.transpose(out, in_, ident), reads=reads, writes=writes)

    def act(self, out, in_, func, reads, writes, **kw):
        nc = self.nc
        return self.op("act", lambda: nc.scalar.activation(out=out, in_=in_, func=func, **kw),
                       reads=reads, writes=writes)

    def tt(self, eng, out, in0, in1, op, reads, writes):
        e = self.engs[eng]
        return self.op(eng, lambda: e.tensor_tensor(out=out, in0=in0, in1=in1, op=op), reads=reads, writes=writes)

    def ts(self, eng, out, in0, s1, op0, reads, writes, s2=None, op1=None, **kw):
        e = self.engs[eng]
        if op1 is None:
            return self.op(eng, lambda: e.tensor_scalar(out=out, in0=in0, scalar1=s1, scalar2=None, op0=op0, **kw),
                           reads=reads, writes=writes)
        return self.op(eng, lambda: e.tensor_scalar(out=out, in0=in0, scalar1=s1, scalar2=s2, op0=op0, op1=op1, **kw),
                       reads=reads, writes=writes)

    def stt(self, eng, out, in0, scalar, in1, op0, op1, reads, writes):
        eng = "dve"
        e = self.engs[eng]
        return self.op(eng, lambda: e.scalar_tensor_tensor(out=out, in0=in0, scalar=scalar, in1=in1, op0=op0, op1=op1),
                       reads=reads, writes=writes)

    def copy(self, eng, out, in_, reads, writes):
        e = self.engs[eng]
        return self.op(eng, lambda: e.tensor_copy(out=out, in_=in_), reads=reads, writes=writes)

    def memset(self, eng, ap, val, writes):
        e = self.engs[eng]
        return self.op(eng, lambda: e.memset(ap, val), writes=writes)


class Ctx:
    NWB = 2

    def __init__(self, nc, P, es, consts):
        self.nc, self.P = nc, P
        self.ones_bf = P.sb(es, "ones_bf", [128, 128], BF16)
        self.ident = P.sb(es, "ident", [128, 128], F32)
        self.ident_bf = P.sb(es, "ident_bf", [128, 128], BF16)
        P.dma("sp", self.ident[:], consts["ident"], "c_ident", writes=["ident"])
        P.memset("dve", self.ones_bf[:], 1.0, writes=["ones_bf"])
        self.epsc = P.sb(es, "epsc", [128, 1], F32)
        P.memset("dve", self.epsc[:], EPS, writes=["epsc"])
        P.copy("dve", self.ident_bf[:], self.ident[:], reads=["ident"], writes=["ident_bf"])
        self.wb = [P.sb(es, f"wbuf{i}", [128, 8192], BF16) for i in range(self.NWB)]
        self.wi = 0
        self.ps = [P.psum(es, f"ps{i}", [128, 512], F32) for i in range(8)]
        self.pi = 0
        self.nrot = 6

    def wbuf_next(self):
        i = self.wi % self.NWB
        self.wi += 1
        return self.wb[i], f"wbuf{i}"

    def ps_next(self):
        i = self.pi % self.nrot
        self.pi += 1
        return self.ps[i], f"ps{i}"


def linear(C, act_fn, act_res_fn, kc, w_dram, m_total, gcols, ntl, evac_fn, post_fn=None):
    P = C.P
    ngroups = m_total // gcols
    views = {}

    def load(g):
        wb, wres = C.wbuf_next()
        view = wb[:, :kc * gcols].rearrange("p (k m) -> p k m", k=kc)
        src = w_dram[:, g * gcols:(g + 1) * gcols].rearrange("(k p) m -> p k m", p=128)
        step = max(1, min(kc, 2048 // max(1, 1)))
        for k0 in range(0, kc, 16):
            k1 = min(kc, k0 + 16)
            P.dma("pool", view[:, k0:k1, :], src[:, k0:k1, :], wres, writes=[wres])
        views[g] = (view, wres)

    load(0)
    pending = None
    for g in range(ngroups):
        if g + 1 < ngroups:
            load(g + 1)
        view, wres = views.pop(g)
        for mi in range(gcols // 128):
            m = g * (gcols // 128) + mi
            banks = [C.ps_next() for _ in ntl]
            for k in range(kc):
                for (n0, nw), (pt, pres) in zip(ntl, banks):
                    P.mm(pt[:, :nw], view[:, k, mi * 128:(mi + 1) * 128], act_fn(k, n0, nw),
                         start=(k == 0), stop=(k == kc - 1),
                         reads=[wres] + act_res_fn(k), writes=[pres])
            if pending is not None and post_fn is not None:
                post_fn(pending)
            for (n0, nw), (pt, pres) in zip(ntl, banks):
                evac_fn(m, n0, nw, pt, pres)
            pending = m
    if pending is not None and post_fn is not None:
        post_fn(pending)


def emit_rstd(C, stat_banks, ntl, rstd, rstd_res, scratch):
    P = C.P
    for (n0, nw), (pt, pres) in zip(ntl, stat_banks):
        P.act(scratch[:, n0:n0 + nw], pt[:, :nw], AF.Sqrt, reads=[pres, "epsc"], writes=[rstd_res + ".t"],
              scale=1.0 / D, bias=C.epsc[:, 0:1])
        P.op("dve", lambda: C.nc.vector.reciprocal(out=rstd[:, n0:n0 + nw], in_=scratch[:, n0:n0 + nw]),
             reads=[rstd_res + ".t"], writes=[rstd_res])


def emit_tail(C, es, *, tag, mixin_fn, mixin_res_fn, nH, ntl, hT, hres, w_out, w1, w2, g_fn, bufs):
    nc, P = C.nc, C.P
    mbuf, aT, Hd, rstd, scratch, sqb, rtmp = (bufs[k] for k in ("mbuf", "aT", "Hd", "rstd", "scratch", "sqb", "rtmp"))
    stat_banks = [(C.ps[6], "ps6"), (C.ps[7], "ps7")][:len(ntl)]
    sqi = [0]

    def stats_evac_factory(dst, dres):
        sq_of = {}

        def evac(m, n0, nw, pt, pres):
            P.act(dst[:, m, n0:n0 + nw], pt[:, :nw], AF.Copy, reads=[pres], writes=[f"{dres}.{m}"])
            i = sqi[0] % len(sqb)
            sqi[0] += 1
            P.tt("pool", sqb[i][:, :nw], dst[:, m, n0:n0 + nw], dst[:, m, n0:n0 + nw], ALU.mult,
                 reads=[f"{dres}.{m}"], writes=[f"sqb{i}"])
            sq_of[(m, n0)] = i

        def post(m):
            for (n0, nw), (pt, pres) in zip(ntl, stat_banks):
                i = sq_of.pop((m, n0))
                P.mm(pt[:, :nw], C.ones_bf[:], sqb[i][:, :nw], start=(m == 0), stop=(m == KC - 1),
                     reads=["ones_bf", f"sqb{i}"], writes=[pres])
        return evac, post

    def residual(src, sres, gi):
        for c in range(KC):
            eng = "pool" if c % 2 else "dve"
            P.tt(eng, src[:, c, :], src[:, c, :], rstd[:, :nH], ALU.mult, reads=[f"{sres}.{c}", "rstd"],
                 writes=[f"{sres}.{c}"])
            P.stt("dve", hT[:, c, :], src[:, c, :], g_fn(gi, c), hT[:, c, :], ALU.mult, ALU.add,
                  reads=[f"{sres}.{c}", f"{hres}.{c}", "gv"], writes=[f"{hres}.{c}"])

    evac, post = stats_evac_factory(mbuf, "mbuf")
    linear(C, mixin_fn, mixin_res_fn, KC, w_out, D, 512, ntl, evac, post)
    emit_rstd(C, stat_banks, ntl, rstd, "rstd", scratch)
    residual(mbuf, "mbuf", 1)
    emit_norm_cast(C, hT, hres, nH, ntl, g_fn, 2, aT, "aT", bufs)
    ri = [0]

    def evac1(m, n0, nw, pt, pres):
        i = ri[0] % len(rtmp)
        ri[0] += 1
        P.act(rtmp[i][:, :nw], pt[:, :nw], AF.Relu, reads=[pres], writes=[f"rtmp{i}"])
        P.tt("pool", Hd[:, m, n0:n0 + nw], rtmp[i][:, :nw], rtmp[i][:, :nw], ALU.mult,
             reads=[f"rtmp{i}"], writes=[f"Hd.{m}"])

    linear(C, lambda k, n0, nw: aT[:, k, n0:n0 + nw], lambda k: [f"aT.{k}"], KC, w1, DFF, 512, ntl, evac1)
    evac, post = stats_evac_factory(mbuf, "mbuf")
    linear(C, lambda k, n0, nw: Hd[:, k, n0:n0 + nw], lambda k: [f"Hd.{k}"], DFF // 128, w2, D, 128, ntl, evac, post)
    emit_rstd(C, stat_banks, ntl, rstd, "rstd", scratch)
    residual(mbuf, "mbuf", 3)


def emit_norm_cast(C, hT, hres, nH, ntl, g_fn, gi, aT, ares, bufs):
    P = C.P
    rstd, scratch, sqb = bufs["rstd"], bufs["scratch"], bufs["sqb"]
    stat_banks = [(C.ps[6], "ps6"), (C.ps[7], "ps7")][:len(ntl)]
    for c in range(KC):
        eng = "pool" if c % 2 else "act"
        sq = bufs["sqfull"][c % len(bufs["sqfull"])]
        sres = f"sqfull{c % len(bufs['sqfull'])}"
        if eng == "act":
            P.act(sq[:, :nH], hT[:, c, :], AF.Square, reads=[f"{hres}.{c}"], writes=[sres])
        else:
            P.tt("pool", sq[:, :nH], hT[:, c, :], hT[:, c, :], ALU.mult, reads=[f"{hres}.{c}"], writes=[sres])
        for (n0, nw), (pt, pres) in zip(ntl, stat_banks):
            P.mm(pt[:, :nw], C.ones_bf[:], sq[:, n0:n0 + nw], start=(c == 0), stop=(c == KC - 1),
                 reads=["ones_bf", sres], writes=[pres])
    emit_rstd(C, stat_banks, ntl, rstd, "rstd", scratch)
    for c in range(KC):
        P.stt("dve", aT[:, c, :], hT[:, c, :], g_fn(gi, c), rstd[:, :nH], ALU.mult, ALU.mult,
              reads=[f"{hres}.{c}", "rstd", "gv"], writes=[f"{ares}.{c}"])


def alloc_tail_bufs(P, es, nH):
    b = {}
    b["mbuf"] = P.sb(es, "mbuf", [128, KC, nH], F32)
    b["aT"] = P.sb(es, "aT", [128, KC, nH], BF16)
    b["Hd"] = P.sb(es, "Hd", [128, DFF // 128, nH], BF16)
    b["rstd"] = P.sb(es, "rstd", [128, nH], F32)
    b["scratch"] = P.sb(es, "rscratch", [128, nH], F32)
    b["sqfull"] = [P.sb(es, f"sqfull{i}", [128, nH], BF16) for i in range(4)]
    b["sqb"] = [P.sb(es, f"sqb{i}", [128, 512], BF16) for i in range(4)]
    b["rtmp"] = [P.sb(es, f"rtmp{i}", [128, 512], BF16) for i in range(4)]
    return b


def load_consts(nc):
    c = {}
    c["ident"] = nc.dram_tensor("c_ident", [128, 128], F32, kind="ExternalInput").ap()
    return c


def host_consts():
    return {"c_ident": np.eye(128, dtype=np.float32)}


def pack_gains(norm_g):
    g = np.asarray(norm_g, dtype=np.float32).reshape(2, 4, KC, 128)
    return np.ascontiguousarray(g.transpose(3, 0, 1, 2).reshape(128, 2 * 4 * KC))


def load_x_half(C, es_unused, xtok, t_base, nH, hT, hres, xin, pst_banks):
    P = C.P
    j0 = 0
    ti = 0
    while j0 < nH:
        tw = min(128, nH - j0)
        xi, xresl = xin[ti % len(xin)]
        xres = xresl[0]
        P.dma("sp", xi[:tw, :], xtok[t_base + j0:t_base + j0 + tw, :], f"xin{ti % len(xin)}", writes=xresl)
        for c4 in range(0, KC, 4):
            pt, pres = C.ps_next()
            for q in range(4):
                c = c4 + q
                P.transpose(pt[:, q * 128:q * 128 + tw], xi[:tw, c * 128:(c + 1) * 128], C.ident[:tw, :tw],
                            reads=[xres, "ident"], writes=[pres])
            P.act(hT[:, c4:c4 + 4, j0:j0 + tw], pt[:, :].rearrange("p (q t) -> p q t", q=4)[:, :, :tw], AF.Copy,
                  reads=[pres], writes=[f"{hres}.{c}" for c in range(c4, c4 + 4)])
        j0 += tw
        ti += 1


def build_phaseB():
    nc = bass.Bass("TRN2", target_bir_lowering=False)
    dt = nc.dram_tensor
    xtok = dt("xtok", [NT, D], F32, kind="ExternalInput").ap()
    ypT = dt("ypT", [1024, NT], BF16, kind="ExternalInput").ap()
    ysT = dt("ysT", [1024, NT], BF16, kind="ExternalInput").ap()
    w_glu = dt("w_glu", [1024, 1024], F32, kind="ExternalInput").ap()
    w_out = dt("w_out", [D, D], F32, kind="ExternalInput").ap()
    w1 = dt("w1", [D, DFF], F32, kind="ExternalInput").ap()
    w2 = dt("w2", [DFF, D], F32, kind="ExternalInput").ap()
    gvd = dt("gvd", [128, 2 * 4 * KC], F32, kind="ExternalInput").ap()
    consts = load_consts(nc)
    h2T = dt("h2T", [D, NT], F32, kind="ExternalOutput").ap()
    a1T = dt("a1T", [D, NT], BF16, kind="ExternalOutput").ap()
    nH = NT // 2
    ntl = [(0, nH // 2), (nH // 2, nH // 2)]
    with ExitStack() as es:
        P = Prog(nc, es)
        C = Ctx(nc, P, es, consts)
        gv = P.sb(es, "gv", [128, 2 * 4 * KC], F32)
        P.dma("sp", gv[:], gvd, "gv", writes=["gv"])
        g_fn = lambda i, c: gv[:, (i * KC + c):(i * KC + c) + 1]
        g_fn1 = lambda i, c: gv[:, ((4 + i) * KC + c):((4 + i) * KC + c) + 1]
        bufs = alloc_tail_bufs(P, es, nH)
        hT = P.sb(es, "hT", [128, KC, nH], F32)
        Hd = bufs["Hd"]
        mixin = Hd[:, 0:KC, :]
        ysg = Hd[:, KC:KC + 8, :]
        xin = [(bufs["mbuf"][:, 4 * i:4 * i + 4, :].rearrange("p c t -> p (c t)")[:, :D],
                [f"mbuf.{c}" for c in range(4 * i, 4 * i + 4)]) for i in range(2)]
        gate = [P.sb(es, f"gate{i}", [128, 512], BF16) for i in range(2)]
        for hf in range(2):
            t0 = hf * nH
            load_x_half(C, es, xtok, t0, nH, hT, "hT", xin, None)
            P.dma("sp", mixin[:, 0:8, :], ypT[:, t0:t0 + nH].rearrange("(c p) t -> p c t", p=128), "mixin_p",
                  writes=[f"Hd.{c}" for c in range(8)])
            P.dma("sp", ysg[:, :, :], ysT[:, t0:t0 + nH].rearrange("(c p) t -> p c t", p=128), "ysg",
                  writes=[f"Hd.{KC + c}" for c in range(8)])
            gi = [0]

            def evac_glu(m, n0, nw, pt, pres):
                i = gi[0] % 2
                gi[0] += 1
                P.act(gate[i][:, :nw], pt[:, :nw], AF.Sigmoid, reads=[pres], writes=[f"gate{i}"])
                P.tt("dve", mixin[:, 8 + m, n0:n0 + nw], ysg[:, m, n0:n0 + nw], gate[i][:, :nw], ALU.mult,
                     reads=[f"gate{i}", f"Hd.{KC + m}"], writes=[f"Hd.{8 + m}"])

            linear(C, lambda k, n0, nw: ysg[:, k, n0:n0 + nw], lambda k: [f"Hd.{KC + k}"], 8, w_glu, 1024, 512, ntl,
                   evac_glu)
            emit_tail(C, es, tag="l0", mixin_fn=lambda k, n0, nw: mixin[:, k, n0:n0 + nw],
                      mixin_res_fn=lambda k: [f"Hd.{k}"], nH=nH, ntl=ntl, hT=hT, hres="hT",
                      w_out=w_out, w1=w1, w2=w2, g_fn=g_fn, bufs=bufs)
            P.dma("sp", h2T[:, t0:t0 + nH].rearrange("(c p) t -> p c t", p=128), hT[:, :, :], "h2T",
                  reads=[f"hT.{c}" for c in range(KC)], writes=["h2T_out"])
            emit_norm_cast(C, hT, "hT", nH, ntl, g_fn1, 0, bufs["aT"], "aT", bufs)
            P.dma("sp", a1T[:, t0:t0 + nH].rearrange("(c p) t -> p c t", p=128), bufs["aT"][:, :, :], "a1T",
                  reads=[f"aT.{c}" for c in range(KC)], writes=["a1T_out"])
        P.barrier()
    return nc


NTL_A = [(0, 512), (512, 512), (1024, 512), (1536, 512), (2048, 16)]
POOL_WINDOWS = (2, 4, 8, 16)


def hostprep_phaseA(inp, b, hc):
    f32 = np.float32
    m = {}
    m["xs"] = np.ascontiguousarray(np.concatenate([inp["meta"], inp["x"][b]], 0), dtype=f32)
    w = inp["ab_w_in"][0]
    m["w_in"] = np.ascontiguousarray(np.concatenate([w[:, hc * 512:(hc + 1) * 512],
                                                     w[:, 1024 + hc * 512:1024 + (hc + 1) * 512]], 1), dtype=f32)
    m["gvd"] = pack_gains(inp["norm_g"])
    m["pool_w"] = np.ascontiguousarray(inp["pool_w"][0][2 * hc:2 * hc + 2], dtype=f32)
    ps = np.asarray(inp["pool_scale"][0][hc * 512:(hc + 1) * 512], dtype=f32)
    m["pool_scale"] = np.ascontiguousarray(ps.reshape(4, 128).T)
    alpha = np.zeros((128, 4, 4), f32)
    invw = np.zeros((128, 4), f32)
    invc = np.zeros((128, 4, 16), f32)
    for i in range(4):
        g = 2 * hc + i // 2
        win = POOL_WINDOWS[g]
        alpha[:, i, g] = 1.0
        invw[:, i] = 1.0 / win
        invc[:, i, :] = 1.0 / np.minimum(np.arange(1, 17), win)
    m["p_alpha"] = alpha.reshape(128, 16)
    m["p_invw"] = invw
    m["p_invc"] = invc.reshape(128, 64)
    gs = slice(32 * hc, 32 * hc + 32)

    def rows(a):
        return np.ascontiguousarray(np.asarray(a, f32).reshape(16, 2, 64).transpose(1, 2, 0).reshape(128, 16))
    m["lam_re"] = rows(inp["s5_lambda_re"][0][gs])
    m["lam_im"] = rows(inp["s5_lambda_im"][0][gs])
    m["log_dt"] = rows(np.repeat(np.asarray(inp["s5_log_dt"][0][gs], f32)[:, None], 64, 1))

    def bpad(bm):
        out = np.zeros((128, 16, 128), f32)
        bm = np.asarray(bm, f32)
        for t in range(16):
            for g2 in range(2):
                c0 = 32 * (t % 4) + 16 * g2
                out[g2 * 64:(g2 + 1) * 64, t, c0:c0 + 16] = bm[2 * t + g2]
        return out
    m["b_re"] = bpad(inp["s5_b_re"][0][gs])
    m["b_im"] = bpad(inp["s5_b_im"][0][gs])

    def cpad(cm):
        return bpad(np.asarray(cm, f32).transpose(0, 2, 1))
    m["c_re"] = cpad(inp["s5_c_re"][0][gs])
    m["c_im"] = cpad(inp["s5_c_im"][0][gs])
    d = np.asarray(inp["s5_d"][0][gs], f32).reshape(4, 128)
    m["s5_d"] = np.ascontiguousarray(d.T)
    m.update(host_consts())
    return m


def build_phaseA():
    nc = bass.Bass("TRN2", target_bir_lowering=False)
    dt = nc.dram_tensor

    def din(name, shape, dty=F32):
        return dt(name, list(shape), dty, kind="ExternalInput").ap()
    xs = din("xs", [T, D])
    w_in = din("w_in", [D, 1024])
    gvd = din("gvd", [128, 2 * 4 * KC])
    pool_w = din("pool_w", [2, 256, 256])
    pool_scale = din("pool_scale", [128, 4])
    p_alpha = din("p_alpha", [128, 16])
    p_invw = din("p_invw", [128, 4])
    p_invc = din("p_invc", [128, 64])
    lam_re = din("lam_re", [128, 16])
    lam_im = din("lam_im", [128, 16])
    log_dt = din("log_dt", [128, 16])
    b_re = din("b_re", [128, 16, 128])
    b_im = din("b_im", [128, 16, 128])
    c_re = din("c_re", [128, 16, 128])
    c_im = din("c_im", [128, 16, 128])
    s5_d = din("s5_d", [128, 4])
    consts = load_consts(nc)
    ypT = dt("ypT", [512, T], BF16, kind="ExternalOutput").ap()
    ysT = dt("ysT", [512, T], BF16, kind="ExternalOutput").ap()
    with ExitStack() as es:
        P = Prog(nc, es)
        C = Ctx(nc, P, es, consts)
        emit_phaseA(C, es, xs, w_in, gvd, pool_w, pool_scale, p_alpha, p_invw, p_invc, lam_re, lam_im, log_dt,
                    b_re, b_im, c_re, c_im, s5_d, ypT, ysT)
        P.barrier()
    return nc


def emit_phaseA(C, es, xs, w_in, gvd, pool_w, pool_scale, p_alpha, p_invw, p_invc, lam_re, lam_im, log_dt,
                b_re, b_im, c_re, c_im, s5_d, ypT, ysT):
    nc, P = C.nc, C.P
    gv = P.sb(es, "gvA", [128, 2 * 4 * KC], F32)
    P.dma("sp", gv[:], gvd, "gvA", writes=["gv"])
    g_fn = lambda i, c: gv[:, (i * KC + c):(i * KC + c) + 1]
    ub = P.sb(es, "ub", [128, 4, T], BF16)
    esu = ExitStack()
    uT = P.sb(esu, "uT", [128, 4, T], F32)
    with ExitStack() as es1:
        a0T = P.sb(es1, "a0T", [128, KC, T], BF16)
        xT = P.sb(es1, "xT", [128, KC, 512], F32)
        xin = [(P.sb(es1, f"xinA{i}", [128, D], F32)[:, :], [f"xinA{i}"]) for i in range(2)]
        nb = {"rstd": P.sb(es1, "rstdA", [128, 512], F32), "scratch": P.sb(es1, "rscrA", [128, 512], F32),
              "sqfull": [P.sb(es1, f"sqfA{i}", [128, 512], BF16) for i in range(4)], "sqb": None}
        for (n0, nw) in NTL_A:
            load_x_half(C, es1, xs, n0, nw, xT[:, :, :nw], "xT", xin, None)
            emit_norm_cast(C, xT[:, :, :nw], "xT", nw, [(0, nw)], g_fn, 0, a0T[:, :, n0:n0 + nw], f"a0T{n0}", nb)
        C.nrot = 8

        def evac_u(m, n0, nw, pt, pres):
            if m < 4:
                P.act(uT[:, m, n0:n0 + nw], pt[:, :nw], AF.Copy, reads=[pres], writes=[f"uT.{m}"])
            else:
                P.act(ub[:, m - 4, n0:n0 + nw], pt[:, :nw], AF.Copy, reads=[pres], writes=[f"ub.{m - 4}"])

        linear(C, lambda k, n0, nw: a0T[:, k, n0:n0 + nw],
               lambda k: [f"a0T{n0}.{k}" for (n0, _) in NTL_A], KC, w_in, 1024, 512, NTL_A, evac_u)
        C.nrot = 6
        P.barrier()
    with ExitStack() as es2:
        sA = P.sb(es2, "poolA", [128, 2, T], F32)
        sB = P.sb(es2, "poolB", [128, 2, T], F32)
        acc = P.sb(es2, "poolacc", [128, 2, T], F32)
        diff = P.sb(es2, "pooldiff", [128, 4, T], BF16)
        ypb = P.sb(es2, "ypb", [128, 4, T], BF16)
        pw = P.sb(es2, "pw", [128, 2, 2, 256], BF16)
        pc = P.sb(es2, "pconst", [128, 16 + 4 + 64 + 4], F32)
        t16 = P.sb(es2, "pt16", [128, 4, 16], F32)
        P.dma("sp", pc[:, 0:16], p_alpha, "pc", writes=["pc"])
        P.dma("sp", pc[:, 16:20], p_invw, "pc", writes=["pc"])
        P.dma("sp", pc[:, 20:84], p_invc, "pc", writes=["pc"])
        P.dma("sp", pc[:, 84:88], pool_scale, "pc", writes=["pc"])
        P.dma("pool", pw[:, :, :, :], pool_w.rearrange("g (k p) m -> p g k m", p=128), "pw", writes=["pw"])
        for i in range(4):
            eng = "dve" if i % 2 == 0 else "pool"
            cur, cres = uT[:, i, :], f"uT.{i}"
            for l in range(1, 5):
                sh = 1 << (l - 1)
                nxt = (sA if l % 2 else sB)[:, i % 2, :]
                nres = f"{'sA' if l % 2 else 'sB'}.{i % 2}"
                P.tt(eng, nxt[:, sh:], cur[:, sh:], cur[:, :T - sh], ALU.add, reads=[cres], writes=[nres])
                P.copy(eng, nxt[:, :sh], cur[:, :sh], reads=[cres], writes=[nres])
                a_col = pc[:, i * 4 + (l - 1):i * 4 + l]
                if l == 1:
                    P.ts(eng, acc[:, i % 2, :], nxt, a_col, ALU.mult, reads=[nres, "pc"], writes=[f"acc.{i % 2}"])
                else:
                    P.stt(eng, acc[:, i % 2, :], nxt, a_col, acc[:, i % 2, :], ALU.mult, ALU.add,
                          reads=[nres, "pc", f"acc.{i % 2}"], writes=[f"acc.{i % 2}"])
                cur, cres = nxt, nres
            P.tt(eng, t16[:, i, :], acc[:, i % 2, 0:16], pc[:, 20 + 16 * i:36 + 16 * i], ALU.mult,
                 reads=[f"acc.{i % 2}", "pc"], writes=[f"t16.{i}"])
            P.stt(eng, diff[:, i, :], acc[:, i % 2, :], pc[:, 16 + i:17 + i], uT[:, i, :], ALU.mult, ALU.subtract,
                  reads=[f"acc.{i % 2}", "pc", f"uT.{i}"], writes=[f"diff.{i}"])
            P.tt(eng, diff[:, i, 0:16], t16[:, i, :], uT[:, i, 0:16], ALU.subtract,
                 reads=[f"t16.{i}", f"uT.{i}", f"diff.{i}"], writes=[f"diff.{i}"])
        for gl in range(2):
            for mc in range(2):
                banks = [C.ps_next() for _ in NTL_A]
                for kc in range(2):
                    for (n0, nw), (pt, pres) in zip(NTL_A, banks):
                        P.mm(pt[:, :nw], pw[:, gl, kc, mc * 128:(mc + 1) * 128], diff[:, 2 * gl + kc, n0:n0 + nw],
                             start=(kc == 0), stop=(kc == 1), reads=["pw", f"diff.{2 * gl + kc}"], writes=[pres])
                mi = 2 * gl + mc
                for (n0, nw), (pt, pres) in zip(NTL_A, banks):
                    P.act(ypb[:, mi, n0:n0 + nw], pt[:, :nw], AF.Copy, reads=[pres, "pc"], writes=[f"ypb.{mi}"],
                          scale=pc[:, 84 + mi:85 + mi])
        P.dma("sp", ypT.rearrange("(c p) t -> p c t", p=128), ypb[:, :, :], "ypT",
              reads=[f"ypb.{i}" for i in range(4)], writes=["ypT_out"])
        P.barrier()
    esu.close()
    with ExitStack() as es3:
        emit_ssm(C, es3, uT, ub, lam_re, lam_im, log_dt, b_re, b_im, c_re, c_im, s5_d, ysT)
        P.barrier()


def emit_gelu_tanh(C, x, xres, out, ores, w, tmp):
    P = C.P
    t1, t2 = tmp
    P.tt("pool", t1[0][:, :w], x, x, ALU.mult, reads=[xres], writes=[t1[1]])
    P.ts("pool", t1[0][:, :w], t1[0][:, :w], 0.044715, ALU.mult, reads=[t1[1]], writes=[t1[1]], s2=1.0, op1=ALU.add)
    P.tt("pool", t2[0][:, :w], t1[0][:, :w], x, ALU.mult, reads=[t1[1], xres], writes=[t2[1]])
    P.act(t1[0][:, :w], t2[0][:, :w], AF.Sigmoid, reads=[t2[1]], writes=[t1[1]], scale=1.5957691216057308)
    P.tt("dve", out, t1[0][:, :w], x, ALU.mult, reads=[t1[1], xres], writes=[ores])


def emit_ssm(C, es, uT, ub, lam_re, lam_im, log_dt, b_re, b_im, c_re, c_im, s5_d, ysT):
    nc, P = C.nc, C.P
    NL = 12
    pp = P.sb(es, "ssm_pp", [128, 48, 16], F32)
    cms = P.sb(es, "ssm_cms", [128, NL, 16], F32)
    sms = P.sb(es, "ssm_sms", [128, NL, 16], F32)
    halfpi = P.sb(es, "halfpi", [128, 1], F32)
    P.memset("dve", halfpi[:], math.pi / 2, writes=["halfpi"])
    slot = {}

    def S(name):
        if name not in slot:
            slot[name] = len(slot)
        k = slot[name]
        return pp[:, k, :], f"pp.{name}"

    def tt(o, a, b, op):
        (oa, orr), (aa, ar), (ba, br) = S(o), S(a), S(b)
        P.tt("dve", oa, aa, ba, op, reads=[ar, br], writes=[orr])

    def ts(o, a, s1, op0, s2=None, op1=None):
        (oa, orr), (aa, ar) = S(o), S(a)
        P.ts("dve", oa, aa, s1, op0, reads=[ar], writes=[orr], s2=s2, op1=op1)

    P.dma("sp", S("lr")[0], lam_re, "ssm_lr", writes=[S("lr")[1]])
    P.dma("sp", S("li")[0], lam_im, "ssm_li", writes=[S("li")[1]])
    P.dma("sp", S("ldt")[0], log_dt, "ssm_ldt", writes=[S("ldt")[1]])
    P.act(S("dt")[0], S("ldt")[0], AF.Exp, reads=[S("ldt")[1]], writes=[S("dt")[1]])
    tt("dre", "dt", "lr", ALU.mult)
    tt("th", "dt", "li", ALU.mult)
    P.act(S("mag")[0], S("dre")[0], AF.Exp, reads=[S("dre")[1]], writes=[S("mag")[1]])
    P.act(S("s0")[0], S("th")[0], AF.Sin, reads=[S("th")[1]], writes=[S("s0")[1]], scale=1.0 / 16)
    P.act(S("c0")[0], S("th")[0], AF.Sin, reads=[S("th")[1], "halfpi"], writes=[S("c0")[1]], scale=1.0 / 16,
          bias=halfpi[:, 0:1])
    for q in range(4):
        tt(f"sq{q}", f"s{q}", f"s{q}", ALU.mult)
        ts(f"c{q + 1}", f"sq{q}", -2.0, ALU.mult, 1.0, ALU.add)
        (oa, orr), (sa, sr), (ca, cr) = S(f"s{q + 1}"), S(f"s{q}"), S(f"c{q}")
        P.stt("dve", oa, sa, 2.0, ca, ALU.mult, ALU.mult, reads=[sr, cr], writes=[orr])
    P.copy("dve", cms[:, 0, :], S("c4")[0], reads=[S("c4")[1]], writes=["cms.0"])
    P.copy("dve", sms[:, 0, :], S("s4")[0], reads=[S("s4")[1]], writes=["sms.0"])
    lt = P.sb(es, "ssm_lt", [128, 16], F32)
    for l in range(NL - 1):
        P.tt("dve", lt[:], sms[:, l, :], sms[:, l, :], ALU.mult, reads=[f"sms.{l}"], writes=["lt"])
        P.ts("dve", cms[:, l + 1, :], lt[:], -2.0, ALU.mult, reads=["lt"], writes=[f"cms.{l + 1}"], s2=1.0, op1=ALU.add)
        P.stt("dve", sms[:, l + 1, :], sms[:, l, :], 2.0, cms[:, l, :], ALU.mult, ALU.mult,
              reads=[f"sms.{l}", f"cms.{l}"], writes=[f"sms.{l + 1}"])
    tt("abr", "mag", "c4", ALU.mult)
    tt("abi", "mag", "s4", ALU.mult)
    ts("zr", "abr", -1.0, ALU.add)
    tt("l2r", "lr", "lr", ALU.mult)
    tt("l2i", "li", "li", ALU.mult)
    tt("den", "l2r", "l2i", ALU.add)
    (ra, rr), (da, dr) = S("rden"), S("den")
    P.op("dve", lambda: nc.vector.reciprocal(out=ra, in_=da), reads=[dr], writes=[rr])
    tt("t1", "zr", "lr", ALU.mult)
    tt("t2", "abi", "li", ALU.mult)
    tt("t3", "t1", "t2", ALU.add)
    tt("fre", "t3", "rden", ALU.mult)
    tt("t4", "abi", "lr", ALU.mult)
    tt("t5", "zr", "li", ALU.mult)
    tt("t6", "t4", "t5", ALU.subtract)
    tt("fim", "t6", "rden", ALU.mult)
    fre, fim, mag = S("fre"), S("fim"), S("mag")
    braw = P.sb(es, "ssm_braw", [128, 2, 16, 128], F32)
    P.dma("sp", braw[:, 0, :, :], b_re, "ssm_braw", writes=["braw"])
    P.dma("sp", braw[:, 1, :, :], b_im, "ssm_braw", writes=["braw"])
    lB = P.sb(es, "ssm_lB", [128, 2, 16, 128], BF16)
    cc = P.sb(es, "ssm_cc", [128, 2, 16, 128], BF16)
    P.dma("pool", cc[:, 0, :, :], c_re, "ssm_cc", writes=["cc"])
    P.dma("pool", cc[:, 1, :, :], c_im, "ssm_cc", writes=["cc"])
    dcol = P.sb(es, "ssm_d", [128, 4], F32)
    P.dma("sp", dcol[:], s5_d, "ssm_d", writes=["dcol"])
    bt = [P.sb(es, f"ssm_bt{i}", [128, 128], F32) for i in range(4)]
    for t in range(16):
        fr, fi = fre[0][:, t:t + 1], fim[0][:, t:t + 1]
        P.ts("dve", bt[0][:], braw[:, 1, t, :], fi, ALU.mult, reads=["braw", fim[1]], writes=["bt0"])
        P.stt("dve", bt[1][:], braw[:, 0, t, :], fr, bt[0][:], ALU.mult, ALU.subtract,
              reads=["braw", fre[1], "bt0"], writes=["bt1"])
        P.ts("dve", bt[2][:], braw[:, 0, t, :], fi, ALU.mult, reads=["braw", fim[1]], writes=["bt2"])
        P.stt("dve", bt[3][:], braw[:, 1, t, :], fr, bt[2][:], ALU.mult, ALU.add,
              reads=["braw", fre[1], "bt2"], writes=["bt3"])
        for ri, src in ((0, 1), (1, 3)):
            P.transpose(C.ps[7][:, ri * 128:(ri + 1) * 128], bt[src][:], C.ident[:], reads=[f"bt{src}", "ident"],
                        writes=["ps7"])
        P.act(lB[:, :, t, :], C.ps[7][:, 0:256].rearrange("p (r m) -> p r m", r=2), AF.Copy, reads=["ps7"],
              writes=["lB"])
    W = 512
    names = ["br", "bi", "m1", "m2", "m3", "m4", "zr", "zi", "p1", "p2", "p3", "p4", "gr0", "gr1", "gi0", "gi1"]
    wk = {n: P.sb(es, "ssm_" + n, [128, W], F32) for n in names}
    hr = P.sb(es, "ssm_hr", [128, W], BF16)
    nhi = P.sb(es, "ssm_nhi", [128, W], BF16)
    tabs = [(P.sb(es, f"ssm_C{i}", [128, T], F32), P.sb(es, f"ssm_S{i}", [128, T], F32)) for i in range(2)]
    tt1 = [P.sb(es, f"ssm_tt{i}", [128, 1024], F32) for i in range(2)]
    ysb = P.sb(es, "ssm_ysb", [128, 4, T], BF16)
    ylin = P.sb(es, "ssm_ylin", [128, W], F32)
    gt = [(P.sb(es, f"ssm_gt{i}", [128, W], F32), f"gt{i}") for i in range(2)]
    for t in range(16):
        ct = t // 4
        Ct, St = tabs[t % 2]
        cres, sres = f"Ctab{t % 2}", f"Stab{t % 2}"
        eng = "pool" if t % 2 else "dve"
        P.memset(eng, Ct[:, 0:1], 1.0, writes=[cres])
        P.memset(eng, St[:, 0:1], 0.0, writes=[sres])
        tA, tB = tt1[0], tt1[1]
        for l in range(NL):
            m = 1 << l
            w = min(m, T - m)
            if w <= 0:
                break
            cm, sm = cms[:, l, t:t + 1], sms[:, l, t:t + 1]
            P.ts(eng, tA[:, :w], St[:, 0:w], sm, ALU.mult, reads=[sres, f"sms.{l}"], writes=["ttA"])
            P.ts(eng, tB[:, :w], Ct[:, 0:w], sm, ALU.mult, reads=[cres, f"sms.{l}"], writes=["ttB"])
            P.stt(eng, Ct[:, m:m + w], Ct[:, 0:w], cm, tA[:, :w], ALU.mult, ALU.subtract,
                  reads=[cres, f"cms.{l}", "ttA"], writes=[cres])
            P.stt(eng, St[:, m:m + w], St[:, 0:w], cm, tB[:, :w], ALU.mult, ALU.add,
                  reads=[sres, f"cms.{l}", "ttB"], writes=[sres])
        magb = mag[0][:, t:t + 1]
        for j, (n0, nw) in enumerate(NTL_A):
            psA, psB = C.ps[5], C.ps[6]
            P.mm(psA[:, :nw], lB[:, 0, t, :], ub[:, ct, n0:n0 + nw], True, True, reads=["lB", f"ub.{ct}"], writes=["ps5"])
            P.mm(psB[:, :nw], lB[:, 1, t, :], ub[:, ct, n0:n0 + nw], True, True, reads=["lB", f"ub.{ct}"], writes=["ps6"])
            P.act(wk["br"][:, :nw], psA[:, :nw], AF.Copy, reads=["ps5"], writes=["br"])
            P.act(wk["bi"][:, :nw], psB[:, :nw], AF.Copy, reads=["ps6"], writes=["bi"])
            Cj, Sj = Ct[:, n0:n0 + nw], St[:, n0:n0 + nw]

            def mul(o, a, tab, tres, e="pool"):
                P.tt(e, wk[o][:, :nw], wk[a][:, :nw], tab, ALU.mult, reads=[a, tres], writes=[o])
            mul("m1", "br", Cj, cres)
            mul("m2", "bi", Sj, sres)
            mul("m3", "bi", Cj, cres)
            mul("m4", "br", Sj, sres)
            P.tt("dve", wk["zr"][:, :nw], wk["m1"][:, :nw], wk["m2"][:, :nw], ALU.add, reads=["m1", "m2"], writes=["zr"])
            P.tt("dve", wk["zi"][:, :nw], wk["m3"][:, :nw], wk["m4"][:, :nw], ALU.subtract, reads=["m3", "m4"], writes=["zi"])
            gr, gi = f"gr{j % 2}", f"gi{j % 2}"
            if j == 0:
                ir, ii = 0.0, 0.0
                rr_, ri_ = [], []
            else:
                pn = NTL_A[j - 1][1]
                ir = wk[f"gr{(j - 1) % 2}"][:, pn - 1:pn]
                ii = wk[f"gi{(j - 1) % 2}"][:, pn - 1:pn]
                rr_, ri_ = [f"gr{(j - 1) % 2}"], [f"gi{(j - 1) % 2}"]
            mb = magb.to_broadcast([128, nw])
            P.op("dve", lambda: nc.vector.tensor_tensor_scan(out=wk[gr][:, :nw], data0=mb, data1=wk["zr"][:, :nw],
                                                             initial=ir, op0=ALU.mult, op1=ALU.add),
                 reads=["zr", mag[1]] + rr_, writes=[gr])
            P.op("dve", lambda: nc.vector.tensor_tensor_scan(out=wk[gi][:, :nw], data0=mb, data1=wk["zi"][:, :nw],
                                                             initial=ii, op0=ALU.mult, op1=ALU.add),
                 reads=["zi", mag[1]] + ri_, writes=[gi])
            mul("p1", gr, Cj, cres)
            mul("p2", gi, Sj, sres)
            mul("p3", gi, Cj, cres)
            mul("p4", gr, Sj, sres, e="dve")
            P.tt("dve", hr[:, :nw], wk["p1"][:, :nw], wk["p2"][:, :nw], ALU.subtract, reads=["p1", "p2"], writes=["hr"])
            P.stt("dve", nhi[:, :nw], wk["p4"][:, :nw], -1.0, wk["p3"][:, :nw], ALU.mult, ALU.subtract,
                  reads=["p3", "p4"], writes=["nhi"])
            first = (t % 4 == 0)
            last = (t % 4 == 3)
            P.mm(C.ps[j][:, :nw], cc[:, 0, t, :], hr[:, :nw], first, False, reads=["cc", "hr"], writes=[f"ps{j}"])
            P.mm(C.ps[j][:, :nw], cc[:, 1, t, :], nhi[:, :nw], False, last, reads=["cc", "nhi"], writes=[f"ps{j}"])
        if t % 4 == 3:
            for j, (n0, nw) in enumerate(NTL_A):
                P.stt("dve", ylin[:, :nw], ub[:, ct, n0:n0 + nw], dcol[:, ct:ct + 1], C.ps[j][:, :nw], ALU.mult,
                      ALU.add, reads=[f"ub.{ct}", "dcol", f"ps{j}"], writes=["ylin"])
                emit_gelu_tanh(C, ylin[:, :nw], "ylin", ysb[:, ct, n0:n0 + nw], f"ysb.{ct}", nw, gt)
    P.dma("sp", ysT.rearrange("(c p) t -> p c t", p=128), ysb[:, :, :], "ysT",
          reads=[f"ysb.{i}" for i in range(4)], writes=["ysT_out"])


NQ = 1024
NQT = 8
N_HEADS = 16
N_KV = 4
IDX_HEADS = 16
TOPK = 256
ROPE_THETA = 500000.0
NEG = -30000.0
NBIS = 16
DBG = int(os.environ.get("DBG_STAGE", "99"))
SKIP = os.environ.get("DBG_SKIP", "").split(",")
KCH = [(i * 128, min(128, T - i * 128)) for i in range((T + 127) // 128)]


def rope_tables(pos, kind):
    pos = np.asarray(pos, np.float32)
    Cc = np.ones((128, len(pos)), np.float32)
    Ss = np.zeros((128, len(pos)), np.float32)
    if kind == "qk":
        half, period = 16, 128
    else:
        half, period = 8, 64
    inv = (np.float32(ROPE_THETA) ** (-np.arange(half, dtype=np.float32) / np.float32(half))).astype(np.float32)
    for p in range(128):
        pp = p % period
        if pp < half:
            ang = pos * inv[pp]
            Cc[p] = np.cos(ang)
            Ss[p] = -np.sin(ang)
        elif pp < 2 * half:
            ang = pos * inv[pp - half]
            Cc[p] = np.cos(ang)
            Ss[p] = np.sin(ang)
    return Cc, Ss


def rope_perm(kind):
    half, period = (16, 128) if kind == "qk" else (8, 64)
    Pm = np.zeros((128, 128), np.float32)
    for m in range(128):
        pp = m % period
        if pp < half:
            Pm[m + half, m] = 1.0
        elif pp < 2 * half:
            Pm[m - half, m] = 1.0
    return Pm


def hostprep_phaseC(inp, a1T_all, half):
    f32 = np.float32
    w = inp["c_w_in"][0]
    m = {}
    qpos0 = NMETA + NQ * half
    m["a1T_all"] = a1T_all
    m["a1T_q"] = np.ascontiguousarray(a1T_all[:, qpos0:qpos0 + NQ])
    m["wq"] = np.ascontiguousarray(w[:, 0:2048], dtype=f32)
    m["wk"] = np.ascontiguousarray(w[:, 2048:2560], dtype=f32)
    m["wv"] = np.ascontiguousarray(w[:, 2560:3072], dtype=f32)
    m["wqi"] = np.ascontiguousarray(w[:, 3072:4096], dtype=f32)
    m["wki2"] = np.ascontiguousarray(np.concatenate([w[:, 4096:4160], w[:, 4096:4160]], 1), dtype=f32)
    m["wwi"] = np.ascontiguousarray(w[:, 4160:4176], dtype=f32)
    kpos = np.arange(T)
    qpos = np.arange(qpos0, qpos0 + NQ)
    ck, sk = rope_tables(kpos, "qk")
    cik, sik = rope_tables(kpos, "idx")
    cq, sq = rope_tables(qpos, "qk")
    ciq, siq = rope_tables(qpos, "idx")
    m["rt_k"] = np.ascontiguousarray(np.stack([ck, sk, cik, sik], 1))
    m["rt_q"] = np.ascontiguousarray(np.stack([cq, sq, ciq, siq], 1))
    m["perm"] = np.ascontiguousarray(np.stack([rope_perm("qk"), rope_perm("idx")], 1))
    cid = np.where(np.arange(T) < NMETA, 0, (np.arange(T) - NMETA) // 64 + 1)
    allowed = cid[None, :] <= cid[qpos][:, None]
    m["amask"] = np.where(allowed, 0.0, -1e30).astype(f32)
    m.update(host_consts())
    return m


def emit_rope(C, pt, pres, nw, Ctab, Stab, tres, perm, out, ores, tmp, rows=128):
    P = C.P
    xb, t1, t2 = tmp
    P.act(xb[0][:rows, :nw], pt[:rows, :nw], AF.Copy, reads=[pres], writes=[xb[1]])
    if "rope_nodve1" in SKIP:
        P.tt("dve", t1[0][:rows, :nw], xb[0][:rows, :nw], Ctab, ALU.mult, reads=[xb[1], tres], writes=[t1[1]])
    else:
        P.tt("dve", t1[0][:rows, :nw], pt[:rows, :nw], Ctab, ALU.mult, reads=[pres, tres, xb[1]], writes=[t1[1]])
    if "rope_nomm" in SKIP:
        P.tt("dve", t2[0][:rows, :nw], xb[0][:rows, :nw], Stab, ALU.mult, reads=[xb[1], tres], writes=[t2[1]])
    else:
        p2, p2res = C.ps_next()
        P.mm(p2[:rows, :nw], perm[:rows, :rows], xb[0][:rows, :nw], True, True, reads=["perm", xb[1]], writes=[p2res])
        P.tt("dve", t2[0][:rows, :nw], p2[:rows, :nw], Stab, ALU.mult, reads=[p2res, tres], writes=[t2[1]])
    P.tt("dve" if "rope_nopool" in SKIP else "pool", out, t1[0][:rows, :nw], t2[0][:rows, :nw], ALU.add,
         reads=[t1[1], t2[1]], writes=[ores])


def build_phaseC():
    nc = bass.Bass("TRN2", target_bir_lowering=False)
    dt = nc.dram_tensor

    def din(name, shape, dty=F32):
        return dt(name, list(shape), dty, kind="ExternalInput").ap()
    a1T_all = din("a1T_all", [D, T], BF16)
    a1T_q = din("a1T_q", [D, NQ], BF16)
    wq, wk, wv, wqi = din("wq", [D, 2048]), din("wk", [D, 512]), din("wv", [D, 512]), din("wqi", [D, 1024])
    wki2, wwi = din("wki2", [D, 128]), din("wwi", [D, 16])
    rt_k, rt_q, perm_d = din("rt_k", [128, 4, T]), din("rt_q", [128, 4, NQ]), din("perm", [128, 2, 128])
    amask = din("amask", [NQ, T])
    consts = load_consts(nc)
    attnT = dt("attnT", [D, NQ], BF16, kind="ExternalOutput").ap()
    with ExitStack() as es:
        P = Prog(nc, es)
        C = Ctx(nc, P, es, consts)
        emit_phaseC(C, es, a1T_all, a1T_q, wq, wk, wv, wqi, wki2, wwi, rt_k, rt_q, perm_d, amask, attnT)
        P.barrier()
    return nc


def emit_phaseC(C, es, a1T_all, a1T_q, wq, wk, wv, wqi, wki2, wwi, rt_k, rt_q, perm_d, amask, attnT_out):
    nc, P = C.nc, C.P
    perm = P.sb(es, "perm_sb", [128, 2, 128], BF16)
    P.dma("pool", perm[:, :, :], perm_d, "perm", writes=["perm"])
    KT = P.sb(es, "KT", [128, N_KV, T], BF16)
    Vaug = P.sb(es, "Vaug", [128, len(KCH), N_KV, 129], BF16)
    kiT = P.sb(es, "kiT", [128, T], BF16)
    if "nomemset" not in SKIP:
        P.memset("dve" if "dvememset" in SKIP else "pool", Vaug[:, :, :, 128:129], 1.0, writes=["Vaug"])
    rtmp = [[(P.sb(es, f"rp{n}{i}", [128, 512], BF16 if n == "xb" else F32), f"rp{n}{i}") for n in ("xb", "t1", "t2")]
            for i in range(2)]
    with ExitStack() as es1:
        rtk = P.sb(es1, "rtk", [128, 4, T], F32)
        P.dma("sp", rtk[:, :, :], rt_k, "rtk", writes=["rtk"])
        wkb, wkres = C.wb[0][:, :], "wbuf0"
        wvb, wvres = C.wb[1][:, :], "wbuf1"
        wkv = wkb.rearrange("p (k m) -> p k m", k=KC)
        wvv = wvb.rearrange("p (k m) -> p k m", k=KC)
        P.dma("pool", wkv, wk.rearrange("(k p) m -> p k m", p=128), wkres, writes=[wkres])
        P.dma("pool", wvv, wv.rearrange("(k p) m -> p k m", p=128), wvres, writes=[wvres])
        wkib = P.sb(es1, "wkib", [128, KC, 128], BF16)
        P.dma("pool", wkib[:, :, :], wki2.rearrange("(k p) m -> p k m", p=128), "wkib", writes=["wkib"])
        a1c = [P.sb(es1, f"a1c{i}", [128, KC, 512], BF16) for i in range(2)]
        ri = 0
        for j, (n0, nw) in enumerate(NTL_A):
            if "noc1loop" in SKIP:
                break
            if "nolast" in SKIP and nw < 512:
                break
            ab, ares = a1c[j % 2], f"a1c{j % 2}"
            P.dma("sp", ab[:, :, :nw], a1T_all[:, n0:n0 + nw].rearrange("(k p) t -> p k t", p=128), ares, writes=[ares])
            for g in range(N_KV + 1):
                pt, pres = C.ps_next()
                for k in range(KC):
                    lhs = wkv[:, k, g * 128:(g + 1) * 128] if g < N_KV else wkib[:, k, :]
                    P.mm(pt[:, :nw], lhs, ab[:, k, :nw], k == 0, k == KC - 1,
                         reads=[wkres if g < N_KV else "wkib", ares], writes=[pres])
                tmp = rtmp[ri % 2]
                ri += 1
                if "norope" in SKIP:
                    P.act(tmp[0][0][:, :nw], pt[:, :nw], AF.Copy, reads=[pres], writes=[tmp[0][1]])
                    continue
                if g < N_KV:
                    emit_rope(C, pt, pres, nw, rtk[:, 0, n0:n0 + nw], rtk[:, 1, n0:n0 + nw], "rtk", perm[:, 0, :],
                              KT[:, g, n0:n0 + nw], f"KT.{g}", tmp)
                else:
                    emit_rope(C, pt, pres, nw, rtk[:, 2, n0:n0 + nw], rtk[:, 3, n0:n0 + nw], "rtk", perm[:, 1, :],
                              kiT[:, n0:n0 + nw], "kiT", tmp)
            for s0 in range(0, nw, 128):
                if "nov" in SKIP:
                    break
                sw = min(128, nw - s0)
                kc = (n0 + s0) // 128
                pt, pres = C.ps_next()
                for k in range(KC):
                    P.mm(pt[:sw, :512], ab[:, k, s0:s0 + sw], wvv[:, k, :], k == 0, k == KC - 1,
                         reads=[wvres, ares], writes=[pres])
                P.act(Vaug[:sw, kc, :, 0:128], pt[:sw, :512].rearrange("p (g d) -> p g d", g=N_KV), AF.Copy,
                      reads=[pres], writes=["Vaug"])
        P.barrier()
    if DBG <= 1:
        return
    QT = P.sb(es, "QT", [128, N_HEADS, NQ], BF16)
    qiT = P.sb(es, "qiT", [128, 8, NQ], BF16)
    wi = P.sb(es, "wi", [128, NQT, IDX_HEADS], F32)
    with ExitStack() as es2:
        rtq = P.sb(es2, "rtq", [128, 4, NQ], F32)
        P.dma("sp", rtq[:, :, :], rt_q, "rtq", writes=["rtq"])
        a1q = P.sb(es2, "a1q", [128, KC, NQ], BF16)
        P.dma("sp", a1q[:, :, :], a1T_q.rearrange("(k p) t -> p k t", p=128), "a1q", writes=["a1q"])
        wwib = P.sb(es2, "wwib", [128, KC, 16], BF16)
        P.dma("pool", wwib[:, :, :], wwi.rearrange("(k p) m -> p k m", p=128), "wwib", writes=["wwib"])
        ntq = [(0, 512), (512, 512)]
        rj = [0]

        def evac_q(m, n0, nw, pt, pres):
            tmp = rtmp[rj[0] % 2]
            rj[0] += 1
            emit_rope(C, pt, pres, nw, rtq[:, 0, n0:n0 + nw], rtq[:, 1, n0:n0 + nw], "rtq", perm[:, 0, :],
                      QT[:, m, n0:n0 + nw], f"QT.{m}", tmp)

        def evac_qi(m, n0, nw, pt, pres):
            tmp = rtmp[rj[0] % 2]
            rj[0] += 1
            emit_rope(C, pt, pres, nw, rtq[:, 2, n0:n0 + nw], rtq[:, 3, n0:n0 + nw], "rtq", perm[:, 1, :],
                      qiT[:, m, n0:n0 + nw], f"qiT.{m}", tmp)

        linear(C, lambda k, n0, nw: a1q[:, k, n0:n0 + nw], lambda k: ["a1q"], KC, wq, 2048, 512, ntq, evac_q)
        linear(C, lambda k, n0, nw: a1q[:, k, n0:n0 + nw], lambda k: ["a1q"], KC, wqi, 1024, 512, ntq, evac_qi)
        w_scale = (IDX_HEADS ** -0.5) * (64 ** -0.5)
        for qt in range(NQT):
            pt, pres = C.ps_next()
            for k in range(KC):
                P.mm(pt[:, :16], a1q[:, k, qt * 128:(qt + 1) * 128], wwib[:, k, :], k == 0, k == KC - 1,
                     reads=["a1q", "wwib"], writes=[pres])
            P.act(wi[:, qt, :], pt[:, :16], AF.Copy, reads=[pres], writes=["wi"], scale=w_scale)
        P.barrier()
    if DBG <= 2:
        return
    with ExitStack() as es3:
        emit_attention(C, es3, KT, Vaug, kiT, QT, qiT, wi, amask, attnT_out)
        P.barrier()


def emit_attention(C, es, KT, Vaug, kiT, QT, qiT, wi, amask, attnT_out):
    nc, P = C.nc, C.P
    att_scale = 128 ** -0.5
    acc = [P.sb(es, f"at_acc{i}", [128, T], F32) for i in range(2)]
    msk = [P.sb(es, f"at_msk{i}", [128, T], F32) for i in range(2)]
    junk = P.sb(es, "at_junk", [128, T], BF16)
    mb = P.sb(es, "at_mb", [128, T], BF16)
    mbT4 = P.sb(es, "at_mbT4", [128, len(KCH), 4, 128], BF16)
    rr = [P.sb(es, f"at_r{i}", [128, 512], F32) for i in range(2)]
    PT = [P.sb(es, f"at_PT{i}", [128, 512], BF16) for i in range(2)]
    osb = P.sb(es, "at_osb", [128, 4, 128], BF16)
    attnTb = [P.sb(es, f"at_attnT{i}", [128, N_HEADS, 128], BF16) for i in range(2)]
    sc = P.sb(es, "at_sc", [128, 64], F32)
    rden = P.sb(es, "at_rden", [128, 4], F32)
    psS = [(C.ps[i], f"ps{i}") for i in range(3)]
    psL = [(C.ps[i], f"ps{i}") for i in (3, 4)]
    psO = [(C.ps[i], f"ps{i}") for i in (5, 6)]
    psT = C.ps[7][:, :].bitcast(BF16)
    si = [0]
    li = [0]
    for qt in range(NQT):
        q0 = qt * 128
        nkc = min(len(KCH), 10 + qt)
        kmax = min(T, nkc * 128)
        A = acc[qt % 2]
        ares = f"acc{qt % 2}"
        M = msk[qt % 2]
        mres = f"msk{qt % 2}"
        P.dma("sp", M[:, :kmax], amask[q0:q0 + 128, 0:kmax], mres, writes=[mres])
        ntk = [(n0, min(512, kmax - n0)) for n0 in range(0, kmax, 512)]
        for (n0, nw) in ntk:
            for h in range(IDX_HEADS):
                pt, pres = psS[si[0] % 3]
                r, rres = rr[si[0] % 2], f"at_r{si[0] % 2}"
                si[0] += 1
                rb = (h % 2) * 64
                P.mm(pt[:, :nw], qiT[rb:rb + 64, h // 2, q0:q0 + 128], kiT[rb:rb + 64, n0:n0 + nw], True, True,
                     reads=[f"qiT.{h // 2}", "kiT"], writes=[pres])
                P.act(r[:, :nw], pt[:, :nw], AF.Relu, reads=[pres], writes=[rres])
                if h == 0:
                    P.ts("dve", A[:, n0:n0 + nw], r[:, :nw], wi[:, qt, h:h + 1], ALU.mult, reads=[rres, "wi"],
                         writes=[ares])
                else:
                    P.stt("dve", A[:, n0:n0 + nw], r[:, :nw], wi[:, qt, h:h + 1], A[:, n0:n0 + nw], ALU.mult, ALU.add,
                          reads=[rres, "wi", ares], writes=[ares])
        if DBG <= 3:
            continue
        amax, lo, mid, cnt, ge, d0 = (sc[:, i:i + 1] for i in range(6))
        hk = sc[:, 8:8 + NBIS]
        P.op("dve", lambda: nc.vector.tensor_reduce(out=amax, in_=A[:, :kmax], axis=AX.X, op=ALU.max,
                                                    apply_absolute_value=True),
             reads=[ares], writes=["sc.amax"])
        P.tt("dve", A[:, :kmax], A[:, :kmax], M[:, :kmax], ALU.add, reads=[ares, mres], writes=[ares])
        P.ts("dve", lo, amax, -1.001, ALU.mult, reads=["sc.amax"], writes=["sc.lo"], s2=-1e-3, op1=ALU.add)
        P.ts("dve", d0, lo, -2.0, ALU.mult, reads=["sc.lo"], writes=["sc.d0"])
        for k in range(NBIS):
            P.ts("dve", hk[:, k:k + 1], d0, 2.0 ** -(k + 1), ALU.mult, reads=["sc.d0"], writes=["sc.hk"])
        for k in range(NBIS):
            P.tt("dve", mid, lo, hk[:, k:k + 1], ALU.add, reads=["sc.lo", "sc.hk"], writes=["sc.mid"])
            P.op("dve", lambda: nc.vector.tensor_scalar(out=junk[:, :kmax], in0=A[:, :kmax], scalar1=mid, scalar2=0.0,
                                                        op0=ALU.is_ge, op1=ALU.add, accum_out=cnt),
                 reads=[ares, "sc.mid"], writes=["junk", "sc.cnt"])
            P.ts("dve", ge, cnt, float(TOPK) - 0.5, ALU.is_ge, reads=["sc.cnt"], writes=["sc.ge"])
            P.stt("dve", lo, ge, hk[:, k:k + 1], lo, ALU.mult, ALU.add, reads=["sc.ge", "sc.hk", "sc.lo"],
                  writes=["sc.lo"])
        if DBG <= 4:
            continue
        P.ts("dve", mb[:, :kmax], A[:, :kmax], lo, ALU.is_lt, reads=[ares, "sc.lo"], writes=["mb"], s2=NEG, op1=ALU.mult)
        for kc in range(nkc):
            k0, kw = KCH[kc]
            P.transpose(psT[:kw, (kc % 8) * 128:(kc % 8) * 128 + 128], mb[:, k0:k0 + kw], C.ident_bf[:, :],
                        reads=["mb", "ident_bf"], writes=["ps7"])
            P.act(mbT4[:kw, kc, :, :], psT[:kw, (kc % 8) * 128:(kc % 8) * 128 + 128].unsqueeze(1).to_broadcast([kw, 4, 128]),
                  AF.Copy, reads=["ps7"], writes=[f"mbT4.{kc}"])
        if DBG <= 5:
            continue
        for g in range(N_KV):
            for kc in range(nkc):
                k0, kw = KCH[kc]
                pl, plres = psL[li[0] % 2]
                pT_, ptres = PT[li[0] % 2], f"at_PT{li[0] % 2}"
                li[0] += 1
                P.mm(pl[:kw, :512], KT[:, g, k0:k0 + kw], QT[:, 4 * g:4 * g + 4, q0:q0 + 128], True, False,
                     reads=[f"KT.{g}"] + [f"QT.{4 * g + x}" for x in range(4)], writes=[plres])
                P.mm(pl[:kw, :512], C.ident_bf[:kw, :kw], mbT4[:kw, kc, :, :], False, True,
                     reads=["ident_bf", f"mbT4.{kc}"], writes=[plres])
                P.act(pT_[:kw, :], pl[:kw, :512], AF.Exp, reads=[plres], writes=[ptres], scale=att_scale)
                for hh in range(4):
                    po, pores = psO[hh // 2]
                    c0 = (hh % 2) * 129
                    P.mm(po[:, c0:c0 + 129], pT_[:kw, hh * 128:(hh + 1) * 128], Vaug[:kw, kc, g, :],
                         kc == 0 and hh % 2 == 0, kc == nkc - 1 and hh % 2 == 1, reads=[ptres, "Vaug"], writes=[pores])
            for hh in range(4):
                po, pores = psO[hh // 2]
                c0 = (hh % 2) * 129
                P.op("dve", lambda: nc.vector.reciprocal(out=rden[:, hh:hh + 1], in_=po[:, c0 + 128:c0 + 129]),
                     reads=[pores], writes=[f"rden.{hh}"])
                P.ts("dve", osb[:, hh, :], po[:, c0:c0 + 128], rden[:, hh:hh + 1], ALU.mult,
                     reads=[pores, f"rden.{hh}"], writes=[f"osb.{hh}"])
            for hh in range(4):
                P.transpose(psT[:, (hh) * 128:(hh + 1) * 128], osb[:, hh, :], C.ident_bf[:, :],
                            reads=[f"osb.{hh}", "ident_bf"], writes=["ps7"])
            P.act(attnTb[qt % 2][:, 4 * g:4 * g + 4, :], psT[:, 0:512].rearrange("p (h q) -> p h q", h=4), AF.Copy,
                  reads=["ps7"], writes=[f"attnT{qt % 2}"])
        P.dma("sp", attnT_out[:, q0:q0 + 128].rearrange("(h p) t -> p h t", p=128), attnTb[qt % 2][:, :, :],
              "attnT_out", reads=[f"attnT{qt % 2}"], writes=["attnT_out"])


def build_phaseD():
    nc = bass.Bass("TRN2", target_bir_lowering=False)
    dt = nc.dram_tensor
    attnT = dt("attnT_in", [D, NQ], BF16, kind="ExternalInput").ap()
    h2q = dt("h2T_q", [D, NQ], F32, kind="ExternalInput").ap()
    w_out = dt("w_out", [D, D], F32, kind="ExternalInput").ap()
    w1 = dt("w1", [D, DFF], F32, kind="ExternalInput").ap()
    w2 = dt("w2", [DFF, D], F32, kind="ExternalInput").ap()
    gvd = dt("gvd", [128, 2 * 4 * KC], F32, kind="ExternalInput").ap()
    consts = load_consts(nc)
    out = dt("out", [NQ, D], F32, kind="ExternalOutput").ap()
    with ExitStack() as es:
        P = Prog(nc, es)
        C = Ctx(nc, P, es, consts)
        emit_phaseD(C, es, attnT, h2q, w_out, w1, w2, gvd, out)
        P.barrier()
    return nc


def emit_phaseD(C, es, attnT, h2q, w_out, w1, w2, gvd, out):
    nc, P = C.nc, C.P
    nH = NQ // 2
    ntl = [(0, nH // 2), (nH // 2, nH // 2)]
    gv = P.sb(es, "gvD", [128, 2 * 4 * KC], F32)
    P.dma("sp", gv[:], gvd, "gvD", writes=["gv"])
    g_fn1 = lambda i, c: gv[:, ((4 + i) * KC + c):((4 + i) * KC + c) + 1]
    bufs = alloc_tail_bufs(P, es, nH)
    hT = P.sb(es, "hTD", [128, KC, nH], F32)
    Hd = bufs["Hd"]
    mixin = Hd[:, 0:KC, :]
    otile = [(bufs["mbuf"][:, 4 * i:4 * i + 4, :].rearrange("p c t -> p (c t)")[:, :D],
              [f"mbuf.{c}" for c in range(4 * i, 4 * i + 4)]) for i in range(2)]
    for hf in range(2):
        t0 = hf * nH
        P.dma("sp", hT[:, :, :], h2q[:, t0:t0 + nH].rearrange("(c p) t -> p c t", p=128), "hTD",
              writes=[f"hT.{c}" for c in range(KC)])
        P.dma("sp", mixin, attnT[:, t0:t0 + nH].rearrange("(c p) t -> p c t", p=128), "mixinD",
              writes=[f"Hd.{c}" for c in range(KC)])
        emit_tail(C, es, tag="l1", mixin_fn=lambda k, n0, nw: mixin[:, k, n0:n0 + nw],
                  mixin_res_fn=lambda k: [f"Hd.{k}"], nH=nH, ntl=ntl, hT=hT, hres="hT",
                  w_out=w_out, w1=w1, w2=w2, g_fn=g_fn1, bufs=bufs)
        for ti, j0 in enumerate(range(0, nH, 128)):
            ot, ores = otile[ti % 2]
            for c4 in range(0, KC, 4):
                pt, pres = C.ps_next()
                for q in range(4):
                    c = c4 + q
                    P.transpose(pt[:, q * 128:(q + 1) * 128], hT[:, c, j0:j0 + 128], C.ident[:, :],
                                reads=[f"hT.{c}", "ident"], writes=[pres])
                P.act(ot[:, c4 * 128:(c4 + 4) * 128], pt[:, :], AF.Copy, reads=[pres], writes=ores)
            P.dma("sp", out[t0 + j0:t0 + j0 + 128, :], ot, "outD", reads=ores, writes=["out_rows"])


_NC_CACHE = {}


def _get_nc(name, fn):
    if name not in _NC_CACHE:
        _NC_CACHE[name] = fn()
    return _NC_CACHE[name]


def kernel(**inputs):
    inp = {k: np.asarray(v) for k, v in inputs.items()}
    B = inp["x"].shape[0]
    ncores = 8
    cores = list(range(ncores))
    gv = pack_gains(inp["norm_g"])
    hc_ = host_consts()
    ncA = build_phaseA()
    mapsA = [hostprep_phaseA(inp, c // 2, c % 2) for c in cores]
    resA = run_bass_kernel_spmd(ncA, mapsA, core_ids=cores).results
    del mapsA
    mapsB = []
    for c in cores:
        b, half = c // 2, c % 2
        yp = np.concatenate([np.asarray(resA[2 * b]["ypT"]), np.asarray(resA[2 * b + 1]["ypT"])], 0)
        ys = np.concatenate([np.asarray(resA[2 * b]["ysT"]), np.asarray(resA[2 * b + 1]["ysT"])], 0)
        hfull = np.concatenate([inp["meta"], inp["x"][b]], 0).astype(np.float32)
        ypl = np.zeros((1024, NT), dtype=yp.dtype)
        ysl = np.zeros((1024, NT), dtype=ys.dtype)
        xt = np.zeros((NT, D), np.float32)
        if half == 0:
            ypl[:], ysl[:], xt[:] = yp[:, :NT], ys[:, :NT], hfull[:NT]
        else:
            ypl[:, NMETA:], ysl[:, NMETA:], xt[NMETA:] = yp[:, NT:], ys[:, NT:], hfull[NT:]
        m = {"xtok": xt, "ypT": ypl, "ysT": ysl,
             "w_glu": np.ascontiguousarray(inp["s5_w_glu"][0], dtype=np.float32),
             "w_out": np.ascontiguousarray(inp["ab_w_out"][0], dtype=np.float32),
             "w1": np.ascontiguousarray(inp["mlp_w1"][0], dtype=np.float32),
             "w2": np.ascontiguousarray(inp["mlp_w2"][0], dtype=np.float32), "gvd": gv}
        m.update(hc_)
        mapsB.append(m)
    del resA
    ncB = build_phaseB()
    resB = run_bass_kernel_spmd(ncB, mapsB, core_ids=cores).results
    del mapsB
    mapsC = []
    for c in cores:
        b, half = c // 2, c % 2
        a1_all = np.concatenate([np.asarray(resB[2 * b]["a1T"])[:, :NT], np.asarray(resB[2 * b + 1]["a1T"])[:, NMETA:]], 1)
        mapsC.append(hostprep_phaseC(inp, np.ascontiguousarray(a1_all), half))
    ncC = build_phaseC()
    resC = run_bass_kernel_spmd(ncC, mapsC, core_ids=cores).results
    del mapsC
    mapsD = []
    for c in cores:
        m = {"attnT_in": np.asarray(resC[c]["attnT"]),
             "h2T_q": np.ascontiguousarray(np.asarray(resB[c]["h2T"])[:, NMETA:]),
             "w_out": np.ascontiguousarray(inp["c_w_out"][0], dtype=np.float32),
             "w1": np.ascontiguousarray(inp["mlp_w1"][1], dtype=np.float32),
             "w2": np.ascontiguousarray(inp["mlp_w2"][1], dtype=np.float32), "gvd": gv}
        m.update(hc_)
        mapsD.append(m)
    ncD = build_phaseD()
    resD = run_bass_kernel_spmd(ncD, mapsD, core_ids=cores).results
    out = np.zeros((B, SEQ, D), np.float32)
    for c in cores:
        b, half = c // 2, c % 2
        out[b, half * NQ:(half + 1) * NQ] = np.asarray(resD[c]["out"])
    return out
```
